# Optimizing a Trainium2 kernel written in Bass

```python
import math
import jax, jax.numpy as jnp
from jax import lax
import numpy as np

D_MODEL = 1024
BATCH = 2
SEQ = 16384
DEPTH = 4

MEM_LEN = 256
HEAD_DIM = 64
EPS = 1e-6
A_WIDTH = D_MODEL // 2
A_HEADS = A_WIDTH // HEAD_DIM
MOBA_BLOCK = 256
MOBA_TOPK = 3
MOBA_QCHUNK = 64
B_HEADS = 4
B_VW = D_MODEL // 4
B_DV = B_VW // B_HEADS
B_DK = B_DV // 2
B_KW = B_HEADS * B_DK
GATE_RANK = 16
GATE_TEMP = 16.0
GLA_CHUNK = 64
M_WIDTH = D_MODEL // 4
M_HEADS = 4
M_DH = M_WIDTH // M_HEADS

IN_SPLITS = (A_WIDTH, A_WIDTH, A_WIDTH, A_WIDTH,
             B_KW, B_KW, B_VW, B_VW, GATE_RANK,
             M_WIDTH, M_WIDTH)
IN_PROJ = sum(IN_SPLITS)

kernel_name = "hymba_moba_gla_mem_hybrid"


def rms_norm(t, g):
    tf = t.astype(jnp.float32)
    y = tf * lax.rsqrt(jnp.mean(tf * tf, axis=-1, keepdims=True) + EPS)
    return (y * g.astype(jnp.float32)).astype(t.dtype)


def split_heads(t, n):
    b, s, w = t.shape
    return t.reshape(b, s, n, w // n).transpose(0, 2, 1, 3)


def merge_heads(t):
    b, h, s, d = t.shape
    return t.transpose(0, 2, 1, 3).reshape(b, s, h * d)


def alibi_slopes(n_heads):
    return jnp.asarray([2.0 ** (-8.0 * (i + 1) / n_heads) for i in range(n_heads)], jnp.float32)


def moba_attention(q, k, v):
    bsz, nh, s, d = q.shape
    nb = -(-s // MOBA_BLOCK)
    sp = nb * MOBA_BLOCK
    pad = ((0, 0), (0, 0), (0, sp - s), (0, 0))
    q, k, v = jnp.pad(q, pad), jnp.pad(k, pad), jnp.pad(v, pad)
    kb = k.reshape(bsz, nh, nb, MOBA_BLOCK, d)
    vb = v.reshape(bsz, nh, nb, MOBA_BLOCK, d)
    kmean = jnp.mean(kb.astype(jnp.float32), axis=3)
    slopes = alibi_slopes(nh)
    scale = d ** -0.5
    nsel = min(MOBA_TOPK, nb)
    n_qc = sp // MOBA_QCHUNK
    qc = q.reshape(bsz, nh, n_qc, MOBA_QCHUNK, d).transpose(2, 0, 1, 3, 4)
    blk_ar = jnp.arange(MOBA_BLOCK, dtype=jnp.int32)
    gather = jax.vmap(jax.vmap(lambda tb, ib: tb[ib]))

    def one_chunk(args):
        qi, c = args
        t0 = c * MOBA_QCHUNK
        qblk = t0 // MOBA_BLOCK
        pos_q = t0 + jnp.arange(MOBA_QCHUNK, dtype=jnp.int32)
        gate = jnp.einsum('bhqd,bhnd->bhqn', qi.astype(jnp.float32), kmean)
        past = jnp.arange(nb, dtype=jnp.int32) < qblk
        gate = jnp.where(past, gate, -jnp.inf)
        _, idx = lax.top_k(gate, nsel)
        valid = idx < qblk
        k_sel = gather(kb, idx)
        v_sel = gather(vb, idx)
        s_past = jnp.einsum('bhqd,bhqjsd->bhqjs', qi, k_sel,
                            preferred_element_type=jnp.float32) * scale
        pos_k_past = idx[..., None] * MOBA_BLOCK + blk_ar
        dist_past = (pos_q[None, None, :, None, None] - pos_k_past).astype(jnp.float32)
        s_past = s_past - slopes[None, :, None, None, None] * dist_past
        s_past = jnp.where(valid[..., None], s_past, -jnp.inf)
        k_own = lax.dynamic_slice_in_dim(kb, qblk, 1, axis=2)[:, :, 0]
        v_own = lax.dynamic_slice_in_dim(vb, qblk, 1, axis=2)[:, :, 0]
        pos_k_own = qblk * MOBA_BLOCK + blk_ar
        dist_own = (pos_q[:, None] - pos_k_own[None, :]).astype(jnp.float32)
        s_own = jnp.einsum('bhqd,bhsd->bhqs', qi, k_own,
                           preferred_element_type=jnp.float32) * scale
        s_own = s_own - slopes[None, :, None, None] * dist_own
        s_own = jnp.where(dist_own >= 0, s_own, -jnp.inf)
        logits = jnp.concatenate([s_past.reshape(bsz, nh, MOBA_QCHUNK, nsel * MOBA_BLOCK), s_own], -1)
        p = jax.nn.softmax(logits, axis=-1).astype(v.dtype)
        p_past = p[..., :nsel * MOBA_BLOCK].reshape(bsz, nh, MOBA_QCHUNK, nsel, MOBA_BLOCK)
        p_own = p[..., nsel * MOBA_BLOCK:]
        return (jnp.einsum('bhqjs,bhqjsd->bhqd', p_past, v_sel)
                + jnp.einsum('bhqs,bhsd->bhqd', p_own, v_own))

    out = lax.map(one_chunk, (qc, jnp.arange(n_qc, dtype=jnp.int32)))
    out = out.transpose(1, 2, 0, 3, 4).reshape(bsz, nh, sp, d)
    return out[:, :, :s]


def gla_attention(q, k, v, log_a):
    bsz, nh, s, dk = q.shape
    dv = v.shape[-1]
    nc = s // GLA_CHUNK

    def to_chunks(t):
        return t.astype(jnp.float32).reshape(bsz, nh, nc, GLA_CHUNK, t.shape[-1]).transpose(2, 0, 1, 3, 4)

    mask = jnp.tril(jnp.ones((GLA_CHUNK, GLA_CHUNK), dtype=bool))

    def step(state, inp):
        qc, kc, vc, gc = inp
        b = jnp.cumsum(gc, axis=-2)
        o_inter = jnp.einsum('bhcd,bhde->bhce', qc * jnp.exp(b), state)
        diff = b[:, :, :, None, :] - b[:, :, None, :, :]
        decay = jnp.exp(jnp.where(mask[None, None, :, :, None], diff, -jnp.inf))
        att = jnp.einsum('bhid,bhjd,bhijd->bhij', qc, kc, decay)
        o_intra = jnp.einsum('bhij,bhje->bhie', att, vc)
        b_last = b[:, :, -1:, :]
        new_state = (jnp.exp(b_last[:, :, 0, :, None]) * state
                     + jnp.einsum('bhcd,bhce->bhde', kc * jnp.exp(b_last - b), vc))
        return new_state, o_inter + o_intra

    state0 = jnp.zeros((bsz, nh, dk, dv), jnp.float32)
    _, out = lax.scan(step, state0, (to_chunks(q), to_chunks(k), to_chunks(v), to_chunks(log_a)))
    out = out.transpose(1, 2, 0, 3, 4).reshape(bsz, nh, s, dv)
    return out.astype(v.dtype)


def mem_attention(q, mk, mv):
    scale = q.shape[-1] ** -0.5
    logits = jnp.einsum('bhqd,bhmd->bhqm', q, mk, preferred_element_type=jnp.float32) * scale
    p = jax.nn.softmax(logits, axis=-1).astype(mv.dtype)
    return jnp.einsum('bhqm,bhmd->bhqd', p, mv)


def setup_inputs(seed: int = 0) -> dict:
    key = jax.random.key(seed)
    ks = jax.random.split(key, 16)
    f32 = jnp.float32

    def gain(k, shape):
        return 1.0 + 0.02 * jax.random.normal(k, shape, f32)

    return {
        "x": jax.random.normal(ks[0], (BATCH, SEQ, D_MODEL), f32),
        "mem": jax.random.normal(ks[1], (BATCH, MEM_LEN, D_MODEL), f32),
        "g_pre": gain(ks[2], (DEPTH, D_MODEL)),
        "w_in": jax.random.normal(ks[3], (DEPTH, D_MODEL, IN_PROJ), f32) * D_MODEL ** -0.5,
        "g_q_moba": gain(ks[4], (DEPTH, HEAD_DIM)),
        "g_k_moba": gain(ks[5], (DEPTH, HEAD_DIM)),
        "w_gate_up": jax.random.normal(ks[6], (DEPTH, GATE_RANK, B_KW), f32) * GATE_RANK ** -0.5,
        "b_gate_up": 0.1 * jax.random.normal(ks[7], (DEPTH, B_KW), f32),
        "g_gla_out": gain(ks[8], (DEPTH, B_DV)),
        "g_mem": gain(ks[9], (DEPTH, D_MODEL)),
        "w_mem_kv": jax.random.normal(ks[10], (DEPTH, D_MODEL, 2 * M_WIDTH), f32) * D_MODEL ** -0.5,
        "g_q_mem": gain(ks[11], (DEPTH, M_DH)),
        "g_k_mem": gain(ks[12], (DEPTH, M_DH)),
        "w_out": jax.random.normal(ks[13], (DEPTH, D_MODEL, D_MODEL), f32) * (D_MODEL * 2 * DEPTH) ** -0.5,
    }


def reference(x, mem, g_pre, w_in, g_q_moba, g_k_moba, w_gate_up, b_gate_up, g_gla_out,
              g_mem, w_mem_kv, g_q_mem, g_k_mem, w_out):
    split_idx = [int(i) for i in np.cumsum(IN_SPLITS)[:-1]]
    for l in range(DEPTH):
        h = rms_norm(x, g_pre[l])
        u = h @ w_in[l]
        (qa, ka, va, ga, qb, kb, vb, gb, rb, qm, gm) = jnp.split(u, split_idx, axis=-1)

        qa_h = rms_norm(split_heads(qa, A_HEADS), g_q_moba[l])
        ka_h = rms_norm(split_heads(ka, A_HEADS), g_k_moba[l])
        oa = merge_heads(moba_attention(qa_h, ka_h, split_heads(va, A_HEADS))) * jax.nn.silu(ga)

        z = rb @ w_gate_up[l] + b_gate_up[l]
        log_a = jax.nn.log_sigmoid(z.astype(jnp.float32)) / GATE_TEMP
        qb_h = split_heads(qb, B_HEADS) * (B_DK ** -0.5)
        ob = gla_attention(qb_h, split_heads(kb, B_HEADS), split_heads(vb, B_HEADS),
                           split_heads(log_a, B_HEADS))
        ob = merge_heads(rms_norm(ob, g_gla_out[l])) * jax.nn.silu(gb)

        mkv = rms_norm(mem, g_mem[l]) @ w_mem_kv[l]
        mk, mv = jnp.split(mkv, 2, axis=-1)
        qm_h = rms_norm(split_heads(qm, M_HEADS), g_q_mem[l])
        mk_h = rms_norm(split_heads(mk, M_HEADS), g_k_mem[l])
        om = merge_heads(mem_attention(qm_h, mk_h, split_heads(mv, M_HEADS))) * jax.nn.silu(gm)

        x = x + jnp.concatenate([oa, ob, om], axis=-1) @ w_out[l]
    return x
```

```python
from contextlib import ExitStack

import numpy as np
import ml_dtypes

import concourse.bass as bass
import concourse.mybir as mybir
from concourse.bass_utils import run_bass_kernel_spmd

F32 = mybir.dt.float32
BF16 = mybir.dt.bfloat16
AF = mybir.ActivationFunctionType
ALU = mybir.AluOpType
AX = mybir.AxisListType

SEQ = 16384
DEPTH = 4
D = 1024
EPS = 1e-6
NWIN = 928
SAME_SYNC = True
NDSEM = 8


class _Stop(Exception):
    pass


class Prog:
    def mark(self, name):
        if self.stop_at is not None and name == self.stop_at:
            raise _Stop()

    def __init__(self):
        self.stop_at = None
        self.ops = []
        self.last_w = {}
        self.readers = {}
        self.alias = {}
        self.epoch = 0

    def _expand(self, names):
        out = []
        for n in names:
            out.extend(self.alias.get(n, (n,)))
        return out

    def op(self, eng, fn, reads=(), writes=(), kind="c", variants=None):
        idx = len(self.ops)
        reads = self._expand(reads)
        writes = self._expand(writes)
        deps = set()
        raw = set()
        for r in reads:
            if r in self.last_w:
                deps.add(self.last_w[r])
                raw.add(self.last_w[r])
            if r[0] == "B" and (r[1:].isdigit() or r == "BT"):
                rd = self.readers.get(r)
                if rd:
                    for en, ix in rd["e"].items():
                        if en != eng:
                            deps.add(ix)
        for w in writes:
            if w in self.last_w:
                deps.add(self.last_w[w])
            rd = self.readers.get(w)
            if rd:
                deps.update(rd["e"].values())
                deps.update(rd["a"])
        async_ = kind != "c"
        for r in reads:
            rd = self.readers.setdefault(r, {"e": {}, "a": []})
            if async_:
                rd["a"].append(idx)
            else:
                rd["e"][eng] = idx
        for w in writes:
            self.last_w[w] = idx
            self.readers[w] = {"e": {}, "a": []}
        deps.discard(idx)
        self.ops.append(dict(eng=eng, fn=fn, deps=deps, raw=raw, kind=kind, variants=variants, ep=self.epoch))
        return idx

    def finalize(self):
        ops = self.ops
        for o in ops:
            o["sig"] = False
        for o in ops:
            for d in o["deps"]:
                w = ops[d]
                if w["kind"] == "c":
                    if w["eng"] != o["eng"] or o["kind"] != "c":
                        w["sig"] = True
                    elif SAME_SYNC and w["eng"] != "pe":
                        w["sig"] = True
        cnt = {}
        dcnt = {}
        dn = {}
        ccn = 0
        for o in ops:
            if o["kind"] == "c":
                ke = (o["eng"], o["ep"])
                if o["sig"]:
                    cnt[ke] = cnt.get(ke, 0) + 1
                o["val"] = cnt.get(ke, 0)
            elif o["kind"] == "dma":
                q = o["eng"]
                n = dn.get(q, 0)
                dn[q] = n + 1
                j = n % NDSEM
                k = dcnt.get((q, j), 0) + 1
                dcnt[(q, j)] = k
                o["dsem"] = (q, j)
                o["val"] = 16 * k
            elif o["kind"] == "cc":
                ccn += 1
                o["val"] = ccn
        self.sig_counts = cnt

    def emit_engine(self, name, e, sems, dsems, ccsem, core=None):
        ops = self.ops
        seen = {}

        def wait(key, sem, val):
            if seen.get(key, 0) >= val:
                return
            seen[key] = val
            e.wait_ge(sem, val)

        for o in ops:
            if o["eng"] != name:
                continue
            dl = sorted(o["deps"], reverse=True)
            for d in dl:
                w = ops[d]
                if w["kind"] == "c":
                    if w["eng"] == name:
                        if o["kind"] == "c" and (name == "pe" or not SAME_SYNC):
                            continue
                    if w["val"] > 0:
                        wait(("c", w["eng"], w["ep"]), sems[(w["eng"], w["ep"])], w["val"])
                elif w["kind"] == "dma":
                    wait(("d",) + w["dsem"], dsems[w["dsem"]], w["val"])
                else:
                    wait(("cc",), ccsem, w["val"])
            if o["kind"] == "c":
                ins = o["fn"](e)
                if o["sig"]:
                    ins.then_inc(sems[(name, o["ep"])], 1)
            elif o["kind"] == "dma":
                ds = dsems[o["dsem"]]
                if o["val"] > 16:
                    wait(("d",) + o["dsem"], ds, o["val"] - 16)
                if o["variants"] is None:
                    o["fn"](e).then_inc(ds, 16)
                else:
                    for cond, f in o["variants"]:
                        with e.If(cond(core)):
                            f(e).then_inc(ds, 16)
            else:
                o["fn"](e).then_inc(ccsem, 1)
        if name in ("sp",):
            for (q, j), s in dsems.items():
                last = 0
                for o in ops:
                    if o["kind"] == "dma" and o["dsem"] == (q, j):
                        last = o["val"]
                if last:
                    wait(("d", q, j), s, last)


def build_program(S=SEQ, L=DEPTH, dbg=None):
    NT = S // 128
    NB = S // 256
    TS = S // 4
    NCH = S // 512
    CPS = TS // 512
    NB2 = 2 * NB
    nc = bass.Bass("TRN2", target_bir_lowering=False)
    P = Prog()
    if dbg is not None and dbg.startswith("stop:"):
        P.stop_at = dbg[5:]

    def din(name, shape, dt):
        return nc.dram_tensor(name, list(shape), dt, kind="ExternalInput").ap()

    xT_own = din("xt_own", [256, S], F32)
    MASK8d = din("mask8", [8, 128], F32)
    MKd = din("mk", [128, 2], F32)
    memd = din("mem", [256, D], F32)
    WIN = din("win", [L, D, NWIN], F32)
    WOUT = din("wout", [L, 2048, 256], F32)
    WMKV = din("wmkv", [L, D, 192], F32)
    WG = din("wg", [L, 33, 64], F32)
    VEC = din("vec", [128, 5 * L], F32)
    GPRE = din("gpre", [128, 8 * L], F32)
    GMEM = din("gmem", [128, 8 * L], F32)
    ATd = din("at", [128, 2 * NT], F32)
    MBI = din("mbinit", [128, 512], BF16)
    KC0 = din("kaugc0", [128, S], BF16)
    KC1 = din("kaugc1", [128, S], BF16)
    IDd = din("ident", [128, 128], BF16)
    OBd = din("onesblk", [128, 128], BF16)
    TRd = din("tri4", [128, 512], BF16)
    UNd = din("uneg", [128, 128], F32)
    SELd = din("sel", [128, 64], F32)
    SELMd = din("selm", [128, 128], F32)
    y = nc.dram_tensor("y", [256, S], F32, kind="ExternalOutput").ap()
    HTown = nc.dram_tensor("htown", [256, S], BF16, kind="Internal").ap()
    HG = nc.dram_tensor("hg", [8 * 256, S], BF16, kind="Internal").ap()
    SSown = nc.dram_tensor("ssown", [1, S], F32, kind="Internal").ap()
    SSG = nc.dram_tensor("ssg", [8, S], F32, kind="Internal").ap()
    OTown = nc.dram_tensor("otown", [256, S], BF16, kind="Internal").ap()
    OG = nc.dram_tensor("og", [8 * 256, S], BF16, kind="Internal").ap()
    dbg_out = None
    if dbg == "ot":
        dbg_out = nc.dram_tensor("dbg", [256, S], BF16, kind="ExternalOutput").ap()
    if dbg == "ht":
        dbg_out = nc.dram_tensor("dbg", [256, S], BF16, kind="ExternalOutput").ap()

    es = ExitStack()
    with es:
        def sb(name, shape, dt):
            return es.enter_context(nc.sbuf_tensor(name, list(shape), dt))

        def ps(name, shape, dt):
            return es.enter_context(nc.psum_tensor(name, list(shape), dt))

        K0 = sb("K0", [128, S], BF16)
        K1 = sb("K1", [128, S], BF16)
        VA = sb("VA", [128, NT, 2, 66], BF16)
        HH = sb("HH", [128, 16, 512], BF16)
        hT = HH[:, 8:16, :]
        ONESF = sb("ONESF", [128, 128], F32)
        MASK8 = sb("MASK8", [8, 128], F32)
        MK = sb("MK", [128, 2], F32)
        SSrow = sb("SSrow", [1, 512], F32)
        SS8 = sb("SS8", [8, 512], F32)
        HT2 = sb("HT2", [128, 2, 512], BF16)
        W = sb("W", [128, 8, NWIN], BF16)
        FA = sb("FA", [128, 1024], F32)
        FB = sb("FB", [128, 1024], F32)
        WST = FB
        WM = sb("WM", [128, 8, 192], BF16)
        WGA = sb("WGA", [33, 64], F32)
        WO_ALIAS = NT * 2 * 66 >= 4096
        if WO_ALIAS:
            WO = VA[:].rearrange("p t h c -> p (t h c)")[:, 0:4096].rearrange("p (k n) -> p k n", n=256)
        else:
            WO = sb("WO", [128, 16, 256], BF16)
        P.alias.update({"L1": ["FA0"], "R1": ["FA1"], "XT": ["FA0", "FA1"],
                        "R2": ["FB0"], "QF": ["FB1"], "XN": ["FB0", "FB1"], "WST": ["FB0", "FB1"]})
        VA_NAMES = ["VA_%d" % c_ for c_ in range(NCH)] + ["VAones"]
        if WO_ALIAS:
            P.alias["WOw"] = ["WO"] + VA_NAMES
            P.alias["VAw"] = ["WO"]
        else:
            P.alias["WOw"] = ["WO"]
            P.alias["VAw"] = []
        MEMNT = sb("MEMNT", [128, 8, 256], BF16)
        MKT = sb("MKT", [128, 256], BF16)
        MVA = sb("MVA", [128, 2, 128], BF16)
        IDT = sb("IDT", [128, 128], BF16)
        OBK = sb("OBK", [128, 128], BF16)
        TRI = sb("TRI", [128, 512], BF16)
        UNEG = sb("UNEG", [128, 128], F32)
        SEL = sb("SEL", [128, 64], F32)
        SELM = sb("SELM", [128, 128], F32)
        ATt = sb("ATt", [128, 2 * NT], F32)
        VECt = sb("VECt", [128, 5 * L], F32)
        GPt = sb("GPt", [128, 8 * L], F32)
        GMt = sb("GMt", [128, 8 * L], F32)
        CC = sb("CC", [128, 4], F32)
        KM = sb("KM", [128, NB2], F32)
        Gt = sb("Gt", [128, 4, 2, NB], F32)
        M8 = sb("M8", [128, 64], F32)
        MB = sb("MB", [128, 4, 128], BF16)
        Qc0 = sb("Qc0", [128, 512], BF16)
        Qc1 = sb("Qc1", [128, 512], BF16)
        SQ1 = sb("SQ1", [128, 512], BF16)
        SQ2 = sb("SQ2", [128, 512], BF16)
        L1 = FA[:, 0:512]
        R1 = FA[:, 512:1024]
        R2 = FB[:, 0:512]
        QF = FB[:, 512:1024]
        KF = sb("KF", [128, 512], F32)
        SG3 = sb("SG3", [128, 512], BF16)
        SG4 = sb("SG4", [128, 512], BF16)
        OSB = [sb("OSB0", [128, 512], F32), sb("OSB1", [128, 512], F32)]
        TO = sb("TO", [128, 512], F32)
        TG = TO
        ST = [sb("ST1", [128, 512], BF16), sb("ST2", [128, 512], BF16)]
        PT = [sb("PT%d" % i, [128, 512], BF16) for i in range(4)]
        RBA = sb("RBA", [33, 512], F32)
        E1 = sb("E1", [128, 256], F32)
        SPt = sb("SPt", [128, 4, 64], F32)
        ENB = sb("ENB", [128, 128], F32)
        KBT = sb("KBT", [128, 4, 32], F32)
        KEp = sb("KEp", [128, 4, 64], BF16)
        VBp = sb("VBp", [128, 4, 128], BF16)
        EBT = sb("EBT", [64, 512], F32)
        ENBT = sb("ENBT", [64, 512], F32)
        A4 = sb("A4", [64, 4], F32)
        QET = sb("QET", [64, 512], BF16)
        KET = sb("KET", [64, 512], BF16)
        ATT = PT[0]
        SF = sb("SF", [64, 64], F32)
        TT = sb("TT", [64, 64], F32)
        SBF = [sb("SBF%d" % i, [64, 128], BF16) for i in range(6)]
        QMN = PT[1]
        PM = [SQ1, SQ2]
        P.alias.update({"ATT": ["PT0"], "QMN": ["PT1"], "PM0": ["SQ1"], "PM1": ["SQ2"], "TG": ["TO"],
                        "JK": ["HB"], "HH": ["HHa", "hT"]})
        SS = sb("SS", [128, 4], F32)
        HB = sb("HB", [128, 1024], BF16)
        XT = FA
        XN = FB
        XT3 = FA[:].rearrange("p (f t) -> p f t", t=512)
        XN3 = FB[:].rearrange("p (f t) -> p f t", t=512)
        JK = HB
        B = [ps("B%d" % i, [128, 512], F32) for i in range(7)]
        BT = ps("BT", [128, 1024], BF16)
        Bn = ["B%d" % i for i in range(7)]

        def mm(out, lhsT, rhs, st, sp_, r, w):
            P.op("pe", lambda e: e.matmul(out, lhsT, rhs, start=st, stop=sp_), r, w)

        def tr(out, in_, r, w):
            P.op("pe", lambda e: e.transpose(out, in_, IDT[:]), list(r) + ["IDT"], w)

        def act(out, in_, func, r, w, bias=None, scale=None, accum=None):
            kw = {}
            if bias is not None:
                kw["bias"] = bias
            if scale is not None:
                kw["scale"] = scale
            if accum is not None:
                kw["accum_out"] = accum
            P.op("act", lambda e: e.activation(out, in_, func, **kw), r, w)

        def acopy(out, in_, r, w):
            P.op("act", lambda e: e.copy(out, in_), r, w)

        def vcopy(out, in_, r, w, eng="dve"):
            P.op(eng, lambda e: e.tensor_copy(out, in_), r, w)

        def tt(out, a, b_, op, r, w, eng="dve"):
            P.op(eng, lambda e: e.tensor_tensor(out, a, b_, op), r, w)

        def ts(out, a, s1, s2, op0, op1, r, w, eng="dve"):
            if op1 is None:
                P.op(eng, lambda e: e.tensor_scalar(out, a, s1, None, op0), r, w)
            else:
                P.op(eng, lambda e: e.tensor_scalar(out, a, s1, s2, op0, op1), r, w)

        def stt(out, a, s, b_, op0, op1, r, w):
            P.op("dve", lambda e: e.scalar_tensor_tensor(out, a, s, b_, op0, op1), r, w)

        def mset(ap, v, w, eng="pool"):
            P.op(eng, lambda e: e.memset(ap, v), (), w)

        def dma(out, in_, r, w):
            P.op("sp", lambda e: e.dma_start(out=out, in_=in_), r, w, kind="dma")

        def dma_if(variants, r, w):
            P.op("sp", None, r, w, kind="dma", variants=variants)

        eps_c = CC[:, 0:1]
        one_c = CC[:, 1:2]

        def rstd_chain(src_ap, dst_ap, lo, hi, r, w, scale):
            act(L1[lo:hi, :], src_ap, AF.Ln, r, ["L1"], bias=eps_c[lo:hi], scale=scale)
            act(dst_ap, L1[lo:hi, :], AF.Exp, ["L1"], w, scale=-0.5)

        for (t, d_, n) in [(IDT, IDd, "IDT"), (OBK, OBd, "OBK"), (TRI, TRd, "TRI"), (UNEG, UNd, "UNEG"),
                           (SEL, SELd, "SEL"), (SELM, SELMd, "SELM"), (ATt, ATd, "ATt"), (VECt, VEC, "VECt"),
                           (GPt, GPRE, "GPt"), (GMt, GMEM, "GMt"), (K0, KC0, "K0c"), (K1, KC1, "K1c")]:
            dma(t[:], d_, (), [n])
        dma(MB[:].rearrange("p s c -> p (s c)"), MBI, (), ["MB"])
        dma(MASK8[:], MASK8d, (), ["MASK8"])
        dma(MK[:], MKd, (), ["MK"])
        mset(ONESF[:], 1.0, ["ONESF"])
        mset(CC[:, 0:1], EPS, ["CC"])
        mset(CC[:, 1:2], 1.0, ["CC"])
        mset(VA[:, :, :, 64:65], 1.0, ["VAones"])
        mset(RBA[:], 0.0, ["RBA"])
        mset(RBA[32:33, :], 1.0, ["RBA"])
        mset(VBp[:], 0.0, ["VBp"])
        mset(KEp[:], 0.0, ["KEp"])
        for i in range(6):
            mset(SBF[i][:], 0.0, ["SBF%d" % i])
        mset(KM[:], 0.0, ["KM"])
        mset(MVA[:], 0.0, ["MVA"])
        mset(MVA[:, :, 0:1], 1.0, ["MVA"])
        ts(VECt[:, 0:L], VECt[:, 0:L], 0.125, None, ALU.mult, None, ["VECt"], ["VECt"])
        ts(VECt[:, 3 * L:4 * L], VECt[:, 3 * L:4 * L], 0.125, None, ALU.mult, None, ["VECt"], ["VECt"])
        MEMT = (XT, XN)
        MEMN = ("XT", "XN")
        for mc in range(2):
            dma(MEMT[mc][:], memd[mc * 128:(mc + 1) * 128, :], (), [MEMN[mc]])
        for mc in range(2):
            act(JK[:], MEMT[mc][:], AF.Square, [MEMN[mc]], ["JK", "SS"], accum=SS[:, mc:mc + 1])
        act(SS[:, 2:4], SS[:, 0:2], AF.Ln, ["SS", "CC"], ["SS"], bias=eps_c, scale=1.0 / D)
        act(SS[:, 2:4], SS[:, 2:4], AF.Exp, ["SS"], ["SS"], scale=-0.5)
        for mc in range(2):
            ts(HB[:], MEMT[mc][:], SS[:, 2 + mc:3 + mc], None, ALU.mult, None, [MEMN[mc], "SS"], ["HB"])
            for kc in range(8):
                tr(BT[:, kc * 128:(kc + 1) * 128], HB[:, kc * 128:(kc + 1) * 128], ["HB"], ["BT"])
            vcopy(MEMNT[:, :, mc * 128:(mc + 1) * 128], BT[:].rearrange("p (k t) -> p k t", t=128),
                  ["BT"], ["MEMNT"])

        def allgather(src, dst, rn, wn):
            P.op("pool", lambda e: e.collective_compute("AllGather", ALU.bypass,
                                                        replica_groups=[list(range(8))],
                                                        ins=[src], outs=[dst]),
                 [rn], [wn], kind="cc")

        def cview(dr, c):
            return dr[:, c * 512:(c + 1) * 512].rearrange("(f p) t -> p f t", p=128)

        def ssq_chunk(c):
            act(XT[:], XN[:], AF.Square, ["XN"], ["XT"])
            for f in range(2):
                mm(B[2][:], ONESF[:], XT3[:, f, :], f == 0, f == 1, ["ONESF", "XT"], ["B2"])
            acopy(SSrow[0:1, :], B[2][0:1, :], ["B2"], ["SSrow"])
            dma(SSown[0:1, c * 512:(c + 1) * 512], SSrow[0:1, :], ["SSrow"], ["SSown"])

        def norm_pass(src, srcname):
            allgather(SSown, SSG, "SSown", "SSG")
            for c in range(NCH):
                dma(XN3, cview(src, c), [srcname % c], ["XN"])
                dma(SS8[0:8, :], SSG[:, c * 512:(c + 1) * 512], ["SSG"], ["SS8"])
                mm(B[3][:], MASK8[0:8, :], SS8[0:8, :], True, True, ["MASK8", "SS8"], ["B3"])
                act(L1, B[3][:], AF.Ln, ["B3", "CC"], ["L1"], bias=eps_c, scale=1.0 / D)
                act(R1, L1, AF.Exp, ["L1"], ["R1"], scale=-0.5)
                for f in range(2):
                    tt(HT2[:, f, :], XN3[:, f, :], R1, ALU.mult, ["XN", "R1"], ["HT2"])
                dma(cview(HTown, c), HT2[:], ["HT2"], ["HTown"])
            allgather(HTown, HG, "HTown", "HG")

        for c in range(NCH):
            dma(XN3, cview(xT_own, c), (), ["XN"])
            ssq_chunk(c)
        norm_pass(xT_own, "xin%d")

        def phase1(l):
            P.epoch = l + 1
            vq = VECt[:, 0 * L + l:0 * L + l + 1]
            vk = VECt[:, 1 * L + l:1 * L + l + 1]
            vgla = VECt[:, 2 * L + l:2 * L + l + 1]
            vqm = VECt[:, 3 * L + l:3 * L + l + 1]
            vkm = VECt[:, 4 * L + l:4 * L + l + 1]
            for kc in range(8):
                dma(WST[:, 0:NWIN], WIN[l, kc * 128:(kc + 1) * 128, :], (), ["WST"])
                ts(W[:, kc, :], WST[:, 0:NWIN], GPt[:, l * 8 + kc:l * 8 + kc + 1], None, ALU.mult, None,
                   ["WST", "GPt"], ["W"])
            for kc in range(8):
                dma(WST[:, 0:192], WMKV[l, kc * 128:(kc + 1) * 128, :], (), ["WST"])
                ts(WM[:, kc, :], WST[:, 0:192], GMt[:, l * 8 + kc:l * 8 + kc + 1], None, ALU.mult, None,
                   ["WST", "GMt"], ["WM"])
            dma(WGA[:], WG[l], (), ["WGA"])
            for kc in range(8):
                mm(B[0][:, 0:256], WM[:, kc, 0:128], MEMNT[:, kc, :], kc == 0, kc == 7, ["WM", "MEMNT"], ["B0"])
            act(SQ1[64:128, 0:256], B[0][64:128, 0:256], AF.Square, ["B0"], ["SQ1"])
            mm(B[1][:, 0:256], OBK[64:128, :], SQ1[64:128, 0:256], True, True, ["OBK", "SQ1"], ["B1"])
            act(L1[64:128, 0:256], B[1][64:128, 0:256], AF.Ln, ["B1", "CC"], ["L1"], bias=eps_c[64:128],
                scale=1.0 / 64)
            act(R1[64:128, 0:256], L1[64:128, 0:256], AF.Exp, ["L1"], ["R1"], scale=-0.5)
            stt(MKT[64:128, :], B[0][64:128, 0:256], vkm[64:128], R1[64:128, 0:256], ALU.mult, ALU.mult,
                ["B0", "R1", "VECt"], ["MKT"])
            for mc in range(2):
                for kc in range(8):
                    mm(B[2][:, mc * 64:(mc + 1) * 64], MEMNT[:, kc, mc * 128:(mc + 1) * 128], WM[:, kc, 128:192],
                       kc == 0, kc == 7, ["WM", "MEMNT"], ["B2"])
                acopy(MVA[:, mc, 64:128], B[2][:, mc * 64:(mc + 1) * 64], ["B2"], ["MVA"])
            if WO_ALIAS and l > 0:
                mset(VA[:, :, :, 64:65], 1.0, ["VAones", "VAw"])
            P.mark("setup1")
            mset(SF[:], 0.0, ["SF"])
            mset(SBF[0][:], 0.0, ["SBF0"])

            for c in range(NCH):
                chunk(l, c, vq, vk, vgla, vqm)
            allgather(OTown, OG, "OTown", "OG")

        def chunk(l, c, vq, vk, vgla, vqm):
            cs = slice(c * 512, (c + 1) * 512)
            rp = (c * 512) // TS
            co = (c * 512) % TS
            for cand in range(2):
                dma(HH[:, cand * 8:(cand + 1) * 8, :],
                    HG[cand * 1024:(cand + 1) * 1024, cs].rearrange("(kc p) t -> p kc t", p=128),
                    ["HG"], [("HHa", "hT")[cand]])
            ts(HH[:, 0:8, :], HH[:, 0:8, :], MK[:, 0:1], None, ALU.mult, None, ["HHa", "MK"], ["HHa"])
            stt(HH[:, 8:16, :], HH[:, 8:16, :], MK[:, 1:2], HH[:, 0:8, :], ALU.mult, ALU.add,
                ["HHa", "hT", "MK"], ["hT"])

            P.mark("A")

            def proj(bank, c0, ncol):
                for kc in range(8):
                    mm(B[bank][0:ncol, :], W[:, kc, c0:c0 + ncol], hT[:, kc, :], kc == 0, kc == 7,
                       ["W", "hT"], [Bn[bank]])

            proj(0, 0, 128)
            proj(1, 128, 128)
            act(SQ1[:], B[0][:], AF.Square, ["B0"], ["SQ1"])
            act(SQ2[:], B[1][:], AF.Square, ["B1"], ["SQ2"])
            mm(B[2][:], OBK[:], SQ1[:], True, True, ["OBK", "SQ1"], ["B2"])
            mm(B[3][:], OBK[:], SQ2[:], True, True, ["OBK", "SQ2"], ["B3"])
            rstd_chain(B[2][:], R1[:], 0, 128, ["B2", "CC"], ["R1"], 1.0 / 64)
            rstd_chain(B[3][:], R2[:], 0, 128, ["B3", "CC"], ["R2"], 1.0 / 64)
            stt(QF[:], B[0][:], vq, R1[:], ALU.mult, ALU.mult, ["B0", "R1", "VECt"], ["QF"])
            stt(KF[:], B[1][:], vk, R2[:], ALU.mult, ALU.mult, ["B1", "R2", "VECt"], ["KF"])
            acopy(Qc0[0:64, :], QF[0:64, :], ["QF"], ["Qc0a"])
            vcopy(Qc1[64:128, :], QF[64:128, :], ["QF"], ["Qc1a"], eng="pool")
            acopy(K0[0:64, cs], KF[0:64, :], ["KF", "K0c"], ["K0_%d" % c])
            vcopy(K1[64:128, cs], KF[64:128, :], ["KF", "K1c"], ["K1_%d" % c], eng="pool")
            P.op("dve", lambda e: e.tensor_reduce(KM[0:64, 2 * c:2 * c + 2],
                                                  KF[0:64, :].rearrange("p (b t) -> p b t", t=256),
                                                  AX.X, ALU.add), ["KF"], ["KM"])
            P.op("dve", lambda e: e.tensor_reduce(KM[64:128, NB + 2 * c:NB + 2 * c + 2],
                                                  KF[64:128, :].rearrange("p (b t) -> p b t", t=256),
                                                  AX.X, ALU.add), ["KF"], ["KM"])
            P.mark("B")
            for s in range(4):
                mm(B[4][:, s * NB2:(s + 1) * NB2], QF[:, s * 128:(s + 1) * 128], KM[:, 0:NB2], True, True,
                   ["QF", "KM"], ["B4"])
            vcopy(Gt[:].rearrange("p s h n -> p (s h n)"), B[4][:, 0:4 * NB2], ["B4"], ["Gt"])
            for half in range(2):
                qb = 2 * c + half
                if qb >= 3:
                    mset(Gt[:, 2 * half:2 * half + 2, :, qb:qb + 1], 1e30, ["Gt"])
                    if qb + 1 < NB:
                        mset(Gt[:, 2 * half:2 * half + 2, :, qb + 1:NB], -1e30, ["Gt"])
                else:
                    mset(Gt[:, 2 * half:2 * half + 2, :, qb:NB], -1e30, ["Gt"])
            for s in range(4):
                for h in range(2):
                    i8 = (s * 2 + h) * 8
                    P.op("dve", lambda e, s=s, h=h, i8=i8: e.max(out=M8[:, i8:i8 + 8], in_=Gt[:, s, h, :]),
                         ["Gt"], ["M8"])
            for s in range(4):
                for h in range(2):
                    i8 = (s * 2 + h) * 8
                    hc = 64 if h == 0 else 0
                    kth = 3 if (2 * c + s // 2) >= 3 else 2
                    ts(MB[:, s, hc:hc + NB - 1], Gt[:, s, h, 0:NB - 1], M8[:, i8 + kth:i8 + kth + 1], -1024.0,
                       ALU.is_lt, ALU.mult, ["Gt", "M8"], ["MB"])
            for s in range(4):
                qb = 2 * c + s // 2
                for h in range(2):
                    hc = 64 if h == 0 else 0
                    if qb < 3 and qb <= NB - 2:
                        mset(MB[:, s, hc + qb:hc + qb + 1], 0.0, ["MB"])
                    if qb < 3 and qb + 1 <= NB - 2:
                        mset(MB[:, s, hc + qb + 1:hc + NB - 1], -1024.0, ["MB"])
            for s in range(4):
                tr(BT[:, s * 128:(s + 1) * 128], MB[:, s, :], ["MB"], ["BT"])
            acopy(Qc1[0:64, :], BT[0:64, 0:512], ["BT"], ["Qc1b"])
            vcopy(Qc0[64:128, :], BT[64:128, 0:512], ["BT"], ["Qc0b"])
            P.mark("C")
            for half in range(2):
                bk = 5 + half
                for s2 in range(2):
                    s = half * 2 + s2
                    for kc in range(8):
                        mm(B[bk][:, s2 * 224:(s2 + 1) * 224], hT[:, kc, s * 128:(s + 1) * 128], W[:, kc, 704:928],
                           kc == 0, kc == 7, ["W", "hT"], [Bn[bk]])
                    t = 4 * c + s
                    acopy(VA[:, t, :, 0:64],
                          B[bk][:, s2 * 224:s2 * 224 + 128].rearrange("p (h d) -> p h d", d=64),
                          [Bn[bk], "VAones"], ["VA_%d" % c, "VAw"])
                    vcopy(VBp[:, s, 64:128], B[bk][:, s2 * 224 + 128:s2 * 224 + 192], [Bn[bk]], ["VBp"])
                    vcopy(KBT[:, s, :], B[bk][:, s2 * 224 + 192:s2 * 224 + 224], [Bn[bk]], ["KBT"])
            P.mark("D")
            for (bank, c0, SG, sgn) in ((2, 256, SG3, "SG3"), (3, 384, SG4, "SG4")):
                proj(bank, c0, 128)
                act(TG[:], B[bank][:], AF.Exp, [Bn[bank]], ["TG"], scale=-1.0)
                act(TG[:], TG[:], AF.Ln, ["TG", "CC"], ["TG"], bias=one_c)
                act(TG[:], TG[:], AF.Exp, ["TG"], ["TG"], scale=-1.0)
                tt(SG[:], B[bank][:], TG[:], ALU.mult, [Bn[bank], "TG"], [sgn])
            proj(0, 512, 128)
            proj(1, 640, 64)
            P.mark("E")
            acopy(RBA[0:16, :], B[1][0:16, :], ["B1"], ["RBA"])
            for s in range(4):
                mm(B[4][:, s * 64:(s + 1) * 64], RBA[0:33, s * 128:(s + 1) * 128], WGA[0:33, :], True, True,
                   ["RBA", "WGA"], ["B4"])
            act(E1[:], B[4][:, 0:256], AF.Exp, ["B4"], ["E1"], scale=-1.0)
            act(SPt[:].rearrange("p s c -> p (s c)"), E1[:], AF.Ln, ["E1", "CC"], ["SPt"], bias=one_c)
            for s in range(4):
                mm(B[5][:, s * 32:(s + 1) * 32], UNEG[:], SPt[:, s, 0:32], True, True, ["UNEG", "SPt"], ["B5"])
            for s in range(4):
                mm(B[6][0:64, s * 128:(s + 1) * 128], SPt[:, s, :], UNEG[:], True, True, ["UNEG", "SPt"], ["B6"])
            act(ENB[:], B[5][:, 0:128], AF.Exp, ["B5"], ["ENB"], scale=-1.0)
            tt(KEp[:, :, 32:64], KBT[:], ENB[:].rearrange("p (s d) -> p s d", d=32), ALU.mult,
               ["KBT", "ENB"], ["KEp"])
            act(EBT[32:64, :], B[6][32:64, :], AF.Exp, ["B6"], ["EBT"])
            act(ENBT[32:64, :], B[6][32:64, :], AF.Exp, ["B6"], ["ENBT"], scale=-1.0)
            act(A4[32:64, :], B[6][32:64, 127:512:128], AF.Exp, ["B6"], ["A4"])
            stt(QET[32:64, :], B[0][32:64, :], 32.0 ** -0.5, EBT[32:64, :], ALU.mult, ALU.mult,
                ["B0", "EBT"], ["QET"])
            tt(KET[32:64, :], B[1][32:64, :], ENBT[32:64, :], ALU.mult, ["B1", "ENBT"], ["KET"])
            for s in range(4):
                sl = slice(s * 128, (s + 1) * 128)
                mm(B[4][:, sl], KET[32:64, sl], QET[32:64, sl], True, True, ["KET", "QET"], ["B4"])
            tt(ATT[:], B[4][:], TRI[:], ALU.mult, ["B4", "TRI"], ["ATT"])
            for s in range(4):
                mm(B[5][0:64, s * 64:(s + 1) * 64], KEp[:, s, :], VBp[:, s, 64:128], True, True,
                   ["KEp", "VBp"], ["B5"])
            for s in range(4):
                t = 4 * c + s
                cur = "SBF%d" % (t % 6)
                nxt = "SBF%d" % ((t + 1) % 6)
                sl = slice(s * 128, (s + 1) * 128)
                mm(B[6][:, sl], SBF[t % 6][32:64, :], QET[32:64, sl], True, False, [cur, "QET"], ["B6"])
                mm(B[6][:, sl], VBp[:, s, :], ATT[:, sl], False, True, ["VBp", "ATT"], ["B6"])
                tt(TT[32:64, :], SF[32:64, :], B[5][32:64, s * 64:(s + 1) * 64], ALU.add, ["SF", "B5"], ["TT"])
                ts(SF[32:64, :], TT[32:64, :], A4[32:64, s:s + 1], None, ALU.mult, None, ["TT", "A4"], ["SF"])
                ts(SBF[(t + 1) % 6][32:64, 64:128], TT[32:64, :], A4[32:64, s:s + 1], None, ALU.mult, None,
                   ["TT", "A4"], [nxt], eng="pool")
            act(SQ1[64:128, :], B[6][64:128, :], AF.Square, ["B6"], ["SQ1"])
            mm(B[5][:], OBK[64:128, :], SQ1[64:128, :], True, True, ["OBK", "SQ1"], ["B5"])
            rstd_chain(B[5][64:128, :], R1[64:128, :], 64, 128, ["B5", "CC"], ["R1"], 1.0 / 64)
            stt(TO[64:128, :], B[6][64:128, :], vgla[64:128], R1[64:128, :], ALU.mult, ALU.mult,
                ["B6", "R1", "VECt"], ["TO"])
            tt(ST[0][64:128, :], TO[64:128, :], SG3[64:128, :], ALU.mult, ["TO", "SG3"], ["ST1b"])
            P.mark("F")
            act(SQ2[64:128, :], B[0][64:128, :], AF.Square, ["B0"], ["SQ2"])
            mm(B[4][:], OBK[64:128, :], SQ2[64:128, :], True, True, ["OBK", "SQ2"], ["B4"])
            rstd_chain(B[4][64:128, :], R2[64:128, :], 64, 128, ["B4", "CC"], ["R2"], 1.0 / 64)
            stt(QMN[64:128, :], B[0][64:128, :], vqm[64:128], R2[64:128, :], ALU.mult, ALU.mult,
                ["B0", "R2", "VECt"], ["QMN"])
            for mc in range(2):
                bk = 4 + mc
                mm(B[bk][:], MKT[64:128, mc * 128:(mc + 1) * 128], QMN[64:128, :], True, True,
                   ["MKT", "QMN"], [Bn[bk]])
                act(PM[mc][:], B[bk][:], AF.Exp, [Bn[bk]], ["PM%d" % mc])
            for mc in range(2):
                mm(B[6][:], MVA[:, mc, :], PM[mc][:], mc == 0, mc == 1, ["MVA", "PM%d" % mc], ["B6"])
            vcopy(OSB[0][:], B[6][:], ["B6"], ["OSB0"])
            mm(B[4][:], SELM[:], OSB[0][:], True, True, ["SELM", "OSB0"], ["B4"])
            act(L1[64:128, :], B[4][64:128, :], AF.Ln, ["B4"], ["L1"])
            act(R2[64:128, :], L1[64:128, :], AF.Exp, ["L1"], ["R2"], scale=-1.0)
            tt(TO[64:128, :], OSB[0][64:128, :], R2[64:128, :], ALU.mult, ["OSB0", "R2"], ["TO"])
            tt(ST[1][64:128, :], TO[64:128, :], SG4[64:128, :], ALU.mult, ["TO", "SG4"], ["ST2b"])
            P.mark("G")
            nkt = 4 * c + 4
            Kt = (K0, K1)
            Qc = (Qc0, Qc1)
            def s_ops(kt):
                rel = kt - 4 * c
                qlo = 0 if rel < 0 else rel * 128
                for h in range(2):
                    sbk = (kt % 2) * 2 + h
                    kn = "K%d_%d" % (h, kt // 4)
                    mm(B[sbk][:, qlo:512], Kt[h][:, kt * 128:(kt + 1) * 128], Qc[h][:, qlo:512], True, True,
                       [kn, "K%dc" % h, "Qc%da" % h, "Qc%db" % h], [Bn[sbk]])

            def pv_ops(kt):
                rel = kt - 4 * c
                qlo = 0 if rel < 0 else rel * 128
                ai = rel + NT - 4
                for h in range(2):
                    sbk = (kt % 2) * 2 + h
                    pt = PT[sbk]
                    ptn = "PT%d" % sbk
                    act(pt[:, qlo:512], B[sbk][:, qlo:512], AF.Exp, [Bn[sbk], "ATt"], [ptn],
                        bias=ATt[:, h * NT + ai:h * NT + ai + 1])
                    if rel >= 0:
                        tt(pt[:, qlo:qlo + 128], pt[:, qlo:qlo + 128], TRI[:, 0:128], ALU.mult,
                           [ptn, "TRI"], [ptn])
                for h in range(2):
                    sbk = (kt % 2) * 2 + h
                    pt = PT[sbk]
                    ptn = "PT%d" % sbk
                    mm(B[4 + h][0:65, qlo:512], VA[:, kt, h, 0:65], pt[:, qlo:512], kt == 0, kt == nkt - 1,
                       ["VA_%d" % (kt // 4), "VAones", ptn], [Bn[4 + h]])

            s_ops(0)
            for kt in range(nkt):
                if kt + 1 < nkt:
                    s_ops(kt + 1)
                pv_ops(kt)
            P.mark("H")
            for h in range(2):
                SGh = (SG3, SG4)[h]
                sgn = ("SG3", "SG4")[h]
                vcopy(OSB[h][0:65, :], B[4 + h][0:65, :], [Bn[4 + h]], ["OSB%d" % h])
                mm(B[h][0:64, :], SEL[0:65, :], OSB[h][0:65, :], True, True, ["SEL", "OSB%d" % h], [Bn[h]])
                act(L1[0:64, :], B[h][0:64, :], AF.Ln, [Bn[h]], ["L1"])
                act(R1[0:64, :], L1[0:64, :], AF.Exp, ["L1"], ["R1"], scale=-1.0)
                tt(TO[0:64, :], OSB[h][0:64, :], R1[0:64, :], ALU.mult, ["OSB%d" % h, "R1"], ["TO"])
                tt(ST[h][0:64, :], TO[0:64, :], SGh[0:64, :], ALU.mult, ["TO", sgn], ["ST%da" % (h + 1)])
            dma(OTown[0:128, cs], ST[0][:], ["ST1a", "ST1b"], ["OTown"])
            dma(OTown[128:256, cs], ST[1][:], ["ST2a", "ST2b"], ["OTown"])

        def phase2(l, last):
            for kc in range(16):
                dma(WST[:, 0:256], WOUT[l, kc * 128:(kc + 1) * 128, :], (), ["WST"])
                vcopy(WO[:, kc, :], WST[:, 0:256], ["WST"], ["WOw"])
            xsrc = xT_own if l == 0 else y
            for c in range(NCH):
                for hf in range(2):
                    dma(HH[:, hf * 8:(hf + 1) * 8, :],
                        OG[hf * 1024:(hf + 1) * 1024, c * 512:(c + 1) * 512].rearrange("(kc p) t -> p kc t", p=128),
                        ["OG"], [("HHa", "hT")[hf]])
                dma(XT3, cview(xsrc, c), ["y%d" % c], ["XT"])
                for hf in range(2):
                    for f in range(2):
                        for k8 in range(8):
                            kc = hf * 8 + k8
                            mm(B[f][:], WO[:, kc, f * 128:(f + 1) * 128], HH[:, kc, :], kc == 0, kc == 15,
                               ["WO", ("HHa", "hT")[hf]], [Bn[f]])
                for f in range(2):
                    tt(XN3[:, f, :], XT3[:, f, :], B[f][:], ALU.add, ["XT", Bn[f]], ["XN"])
                dma(cview(y, c), XN3, ["XN"], ["y%d" % c])
                if not last:
                    ssq_chunk(c)
            if not last:
                norm_pass(y, "y%d")

        try:
            if dbg == "ht":
                dma(dbg_out, HTown, ["HTown", "HG"], ["dbg"])
            else:
                for l in range(L):
                    phase1(l)
                    if dbg == "ot":
                        dma(dbg_out, OTown, ["OTown", "OG"], ["dbg"])
                        break
                    phase2(l, l == L - 1)
        except _Stop:
            pass

        P.finalize()
        sems = {}
        for n in ("pe", "act", "dve", "pool"):
            for ep in range(P.epoch + 1):
                sems[(n, ep)] = es.enter_context(nc.semaphore("s_%s%d" % (n, ep)))
        dsems = {}
        for j in range(NDSEM):
            dsems[("sp", j)] = es.enter_context(nc.semaphore("d_sp%d" % j))
        ccsem = es.enter_context(nc.semaphore("ccsem"))
        block = es.enter_context(nc.Block())

        @block.sync
        def _(e):
            core = e.partition_id()
            P.emit_engine("sp", e, sems, dsems, ccsem, core)

        @block.tensor
        def _(e):
            P.emit_engine("pe", e, sems, dsems, ccsem)

        @block.scalar
        def _(e):
            P.emit_engine("act", e, sems, dsems, ccsem)

        @block.vector
        def _(e):
            P.emit_engine("dve", e, sems, dsems, ccsem)

        @block.gpsimd
        def _(e):
            P.emit_engine("pool", e, sems, dsems, ccsem)

    return nc, P


def prep_inputs(inp, S=SEQ, L=DEPTH):
    bf = ml_dtypes.bfloat16
    NT = S // 128
    NB = S // 256
    TS = S // 4
    f = lambda a: np.ascontiguousarray(np.asarray(a, dtype=np.float32))
    x = f(inp["x"])
    mem = f(inp["mem"])
    w_in = f(inp["w_in"])[:L]
    w_out = f(inp["w_out"])[:L]
    w_mem_kv = f(inp["w_mem_kv"])[:L]
    w_gate_up = f(inp["w_gate_up"])[:L]
    b_gate_up = f(inp["b_gate_up"])[:L]
    g_pre = f(inp["g_pre"])[:L]
    g_mem = f(inp["g_mem"])[:L]
    gq = f(inp["g_q_moba"])[:L]
    gk = f(inp["g_k_moba"])[:L]
    ggla = f(inp["g_gla_out"])[:L]
    gqm = f(inp["g_q_mem"])[:L]
    gkm = f(inp["g_k_mem"])[:L]

    p = np.arange(128)
    ident = np.eye(128, dtype=np.float32).astype(bf)
    onesblk = np.zeros((128, 128), np.float32)
    onesblk[:64, :64] = 1
    onesblk[64:, 64:] = 1
    onesblk = onesblk.astype(bf)
    tri = (p[:, None] <= p[None, :]).astype(np.float32)
    tri4 = np.concatenate([tri] * 4, axis=1).astype(bf)
    uneg = (-tri / 16.0).astype(np.float32)
    sel = np.zeros((128, 64), np.float32)
    sel[64, :] = 1
    selm = np.zeros((128, 128), np.float32)
    selm[0, 64:] = 1
    kblk = np.arange(S) // 256
    kc0 = np.zeros((128, S), np.float32)
    kc1 = np.zeros((128, S), np.float32)
    for j in range(min(NB, 63)):
        kc0[64 + j, kblk == j] = 1
        kc1[j, kblk == j] = 1
    kc0[127, :] = 1
    kc1[63, :] = 1
    kc0 = kc0.astype(bf)
    kc1 = kc1.astype(bf)
    gpre_l = np.ascontiguousarray(g_pre.reshape(L, 8, 128).transpose(2, 0, 1).reshape(128, 8 * L))
    gmem_l = np.ascontiguousarray(g_mem.reshape(L, 8, 128).transpose(2, 0, 1).reshape(128, 8 * L))
    perm = []
    for g in range(4):
        perm += list(range(2 * g * 64, 2 * g * 64 + 64))
        perm += list(range(512 + g * 64, 512 + g * 64 + 64))
        perm += list(range((2 * g + 1) * 64, (2 * g + 1) * 64 + 64))
        perm += list(range(768 + g * 64, 768 + g * 64 + 64))
    wout_p = np.ascontiguousarray(w_out[:, perm, :])
    xT = [np.ascontiguousarray(x[b].T) for b in range(2)]

    maps = []
    for core in range(8):
        b, g = core // 4, core % 4
        h0, h1 = 2 * g, 2 * g + 1
        A = lambda base, h: w_in[:, :, base + h * 64: base + (h + 1) * 64]
        z = lambda n: np.zeros((L, D, n), np.float32)
        qb = w_in[:, :, 2048 + g * 32:2048 + (g + 1) * 32]
        kb = w_in[:, :, 2176 + g * 32:2176 + (g + 1) * 32]
        vb = w_in[:, :, 2304 + g * 64:2304 + (g + 1) * 64]
        gb = w_in[:, :, 2560 + g * 64:2560 + (g + 1) * 64]
        rb = w_in[:, :, 2816:2832]
        qm = w_in[:, :, 2832 + g * 64:2832 + (g + 1) * 64]
        gm = w_in[:, :, 3088 + g * 64:3088 + (g + 1) * 64]
        win = np.concatenate([
            A(0, h0), A(0, h1),
            A(512, h0), A(512, h1),
            A(1536, h0), gb,
            A(1536, h1), gm,
            z(32), qb, qm,
            rb, z(16), kb,
            A(1024, h0), A(1024, h1), vb, kb,
        ], axis=2)
        assert win.shape[2] == NWIN
        wmkv = np.concatenate([z(64), w_mem_kv[:, :, g * 64:(g + 1) * 64],
                               w_mem_kv[:, :, 256 + g * 64:256 + (g + 1) * 64]], axis=2)
        wg = np.zeros((L, 33, 64), np.float32)
        wgh = w_gate_up[:, :, g * 32:(g + 1) * 32]
        wg[:, 0:16, 0:32] = wgh
        wg[:, 0:16, 32:64] = wgh
        wg[:, 32, 0:32] = b_gate_up[:, g * 32:(g + 1) * 32]
        wg[:, 32, 32:64] = b_gate_up[:, g * 32:(g + 1) * 32]
        vec = np.zeros((128, 5, L), np.float32)
        vec[:, 0, :] = np.concatenate([gq.T, gq.T], axis=0)
        vec[:, 1, :] = np.concatenate([gk.T, gk.T], axis=0)
        vec[64:, 2, :] = ggla.T
        vec[64:, 3, :] = gqm.T
        vec[64:, 4, :] = gkm.T
        sl = [2.0 ** (-(h0 + 1)), 2.0 ** (-(h1 + 1))]
        at = np.zeros((128, 2, NT), np.float32)
        idx = np.arange(NT)
        for h in range(2):
            at[:, h, :] = sl[h] * (128.0 * (idx[None, :] - NT + 4) + p[:, None])
        mbi = np.zeros((128, 4, 128), np.float32)
        for s in range(4):
            mbi[:, s, 127] = -sl[0] * (128 * s + p)
            mbi[:, s, 63] = -sl[1] * (128 * s + p)
        wext = np.zeros((L, 2048, 256), np.float32)
        wext[:, b * 1024:(b + 1) * 1024, :] = wout_p[:, :, g * 256:(g + 1) * 256]
        mask8 = np.zeros((8, 128), np.float32)
        mask8[4 * b:4 * b + 4, :] = 1.0
        mk = np.zeros((128, 2), np.float32)
        mk[:, b] = 1.0
        maps.append({
            "xt_own": np.ascontiguousarray(xT[b][g * 256:(g + 1) * 256, :]),
            "mask8": mask8,
            "mk": mk,
            "mem": np.ascontiguousarray(mem[b]),
            "win": np.ascontiguousarray(win),
            "wout": wext,
            "wmkv": np.ascontiguousarray(wmkv),
            "wg": wg,
            "vec": np.ascontiguousarray(vec.reshape(128, 5 * L)),
            "gpre": gpre_l,
            "gmem": gmem_l,
            "at": np.ascontiguousarray(at.reshape(128, 2 * NT)),
            "mbinit": np.ascontiguousarray(mbi.reshape(128, 512)).astype(bf),
            "kaugc0": kc0,
            "kaugc1": kc1,
            "ident": ident,
            "onesblk": onesblk,
            "tri4": tri4,
            "uneg": uneg,
            "sel": sel,
            "selm": selm,
        })
    return maps


_CACHE = {}


def kernel(**inputs):
    S = int(np.asarray(inputs["x"]).shape[1])
    L = int(np.asarray(inputs["w_in"]).shape[0])
    key = (S, L)
    if key not in _CACHE:
        _CACHE[key] = build_program(S, L)[0]
    nc = _CACHE[key]
    maps = prep_inputs(inputs, S, L)
    res = run_bass_kernel_spmd(nc, maps, core_ids=list(range(8)))
    out = np.empty((2, S, D), np.float32)
    for core in range(8):
        b, g = core // 4, core % 4
        out[b, :, g * 256:(g + 1) * 256] = np.asarray(res.results[core]["y"]).T
    return out
```
